# Optimizing a Trainium2 kernel written in Bass

```python
import jax, jax.numpy as jnp
from jax import lax

D_MODEL = 4096
BATCH = 2
SEQ = 4096
DEPTH = 2

CTX_LEN = 256
GRID_W = 64
HEAD_DIM = 128
NA_HEADS = 12
NA_WIN_H = 8
NA_WIN_W = 16
GQ_HEADS = 16
GQ_KV_HEADS = 4
Q_BLOCK = 128
ROPE_THETA = 10000.0
LRU_WIDTH = 1536
LRU_BLOCKS = 12
CONV_W = 4
LRU_C = 8.0
N_BRANCH = 3
D_FF = 11008
N_EXPERTS = 8
TOP_K = 2
D_FF_EXPERT = 3072
N_DENSE = (DEPTH + 1) // 2
N_MOE = DEPTH // 2
ALPHA = (2 * DEPTH) ** 0.25
BETA = (8 * DEPTH) ** -0.25
EPS = 1e-6
NEG_INF = -1e30

NA_WIDTH = NA_HEADS * HEAD_DIM
GQ_Q_WIDTH = GQ_HEADS * HEAD_DIM
GQ_KV_WIDTH = GQ_KV_HEADS * HEAD_DIM
LRU_BLOCK = LRU_WIDTH // LRU_BLOCKS
IN_SPLITS = (NA_WIDTH, NA_WIDTH, NA_WIDTH, GQ_Q_WIDTH, GQ_KV_WIDTH, GQ_KV_WIDTH, LRU_WIDTH, LRU_WIDTH, N_BRANCH * D_MODEL)
IN_COLS = sum(IN_SPLITS)

kernel_name = 'hybrid_natten_gqa_rglru_moe_dit'


def layer_norm(x, g, b):
    xf = x.astype(jnp.float32)
    mu = jnp.mean(xf, axis=-1, keepdims=True)
    var = jnp.mean(jnp.square(xf - mu), axis=-1, keepdims=True)
    return ((xf - mu) * lax.rsqrt(var + EPS) * g.astype(jnp.float32) + b.astype(jnp.float32)).astype(x.dtype)


def rms_norm(x, g):
    xf = x.astype(jnp.float32)
    return (xf * lax.rsqrt(jnp.mean(jnp.square(xf), axis=-1, keepdims=True) + EPS) * g.astype(jnp.float32)).astype(x.dtype)


def split_cols(p):
    parts = []
    start = 0
    for width in IN_SPLITS:
        parts.append(p[..., start:start + width])
        start += width
    return parts


def heads(t, n):
    return t.reshape(t.shape[0], t.shape[1], n, HEAD_DIM)


def rope_1d(x, pos):
    half = x.shape[-1] // 2
    inv = ROPE_THETA ** (-jnp.arange(half, dtype=jnp.float32) / half)
    ang = pos.astype(jnp.float32)[:, None] * inv[None, :]
    cos = jnp.cos(ang)[:, None, :]
    sin = jnp.sin(ang)[:, None, :]
    xf = x.astype(jnp.float32)
    x1, x2 = xf[..., :half], xf[..., half:]
    return jnp.concatenate([x1 * cos - x2 * sin, x2 * cos + x1 * sin], axis=-1).astype(x.dtype)


def rope_2d(x):
    t = jnp.arange(x.shape[1])
    hd2 = HEAD_DIM // 2
    return jnp.concatenate([rope_1d(x[..., :hd2], t // GRID_W), rope_1d(x[..., hd2:], t % GRID_W)], axis=-1)


def attend(q, k, v):
    B, L, H, d = q.shape
    KV = k.shape[2]
    qg = q.reshape(B, L, KV, H // KV, d)
    s = jnp.einsum('blkgd,bmkd->bkglm', qg, k).astype(jnp.float32) * (d ** -0.5)
    p = jax.nn.softmax(s, axis=-1).astype(v.dtype)
    return jnp.einsum('bkglm,bmkd->blkgd', p, v).reshape(B, L, H * d)


def blocked_attention(q, k_all, v_all):
    B, L, H, d = q.shape
    nblk = L // Q_BLOCK
    qb = jnp.swapaxes(q.reshape(B, nblk, Q_BLOCK, H, d), 0, 1)
    out = lax.map(lambda qq: attend(qq, k_all, v_all), qb)
    return jnp.swapaxes(out, 0, 1).reshape(B, L, H * d)


def natten_latent(q, k, v, k_ctx, v_ctx, rpb):
    B, S, H, d = q.shape
    rows = S // GRID_W
    wh = min(NA_WIN_H, rows)
    r = jnp.arange(rows)
    rs = jnp.clip(r - wh // 2, 0, rows - wh)
    row_idx = rs[:, None] + jnp.arange(wh)[None, :]
    roff = row_idx - r[:, None] + (NA_WIN_H - 1)
    cols = jnp.arange(GRID_W)
    cs = jnp.clip(cols - NA_WIN_W // 2, 0, GRID_W - NA_WIN_W)
    valid = (cols[None, :] >= cs[:, None]) & (cols[None, :] < cs[:, None] + NA_WIN_W)
    coff = jnp.clip(cols[None, :] - cols[:, None], -(NA_WIN_W - 1), NA_WIN_W - 1) + (NA_WIN_W - 1)
    bias = rpb.astype(jnp.float32)[:, roff[:, None, :, None], coff[None, :, None, :]]
    qg = q.reshape(B, rows, GRID_W, H, d)
    kg = k.reshape(B, rows, GRID_W, H, d)[:, row_idx]
    vg = v.reshape(B, rows, GRID_W, H, d)[:, row_idx]
    scale = d ** -0.5
    s_lat = jnp.einsum('brqhd,brwkhd->bhrqwk', qg, kg).astype(jnp.float32) * scale + bias[None]
    s_lat = jnp.where(valid[:, None, :], s_lat, NEG_INF).reshape(B, H, rows, GRID_W, wh * GRID_W)
    s_ctx = jnp.einsum('brqhd,bchd->bhrqc', qg, k_ctx).astype(jnp.float32) * scale
    p = jax.nn.softmax(jnp.concatenate([s_lat, s_ctx], axis=-1), axis=-1).astype(v.dtype)
    n_lat = wh * GRID_W
    p_lat = p[..., :n_lat].reshape(B, H, rows, GRID_W, wh, GRID_W)
    out = jnp.einsum('bhrqwk,brwkhd->brqhd', p_lat, vg) + jnp.einsum('bhrqc,bchd->brqhd', p[..., n_lat:], v_ctx)
    return out.reshape(B, S, H * d)


def centred_conv(x, w, b):
    L = x.shape[1]
    xp = jnp.pad(x, ((0, 0), (CONV_W // 2, CONV_W - 1 - CONV_W // 2), (0, 0)))
    out = b
    for tap in range(CONV_W):
        out = out + w[tap] * xp[:, tap:tap + L]
    return out


def rglru_coeffs(xc, w_r, b_r, w_i, b_i, lam):
    B, L, W = xc.shape
    xb = xc.reshape(B, L, LRU_BLOCKS, LRU_BLOCK)
    gate_r = jax.nn.sigmoid(jnp.einsum('blgi,gij->blgj', xb, w_r).reshape(B, L, W) + b_r).astype(jnp.float32)
    gate_i = jax.nn.sigmoid(jnp.einsum('blgi,gij->blgj', xb, w_i).reshape(B, L, W) + b_i).astype(jnp.float32)
    log_a = -LRU_C * gate_r * jax.nn.softplus(-lam.astype(jnp.float32))
    a = jnp.exp(log_a)
    b = jnp.sqrt(-jnp.expm1(2.0 * log_a)) * (gate_i * xc.astype(jnp.float32))
    return a, b


def scan_combine(e1, e2):
    a1, b1 = e1
    a2, b2 = e2
    return a1 * a2, a2 * b1 + b2


def linear_scan(a, b, h0):
    b = b.at[:, 0].add(a[:, 0] * h0)
    _, h = lax.associative_scan(scan_combine, (a, b), axis=1)
    return h


def bidirectional_rglru(xc_x, xc_c, w_r, b_r, w_i, b_i, lam):
    B = xc_x.shape[0]
    h0 = jnp.zeros((B, LRU_WIDTH), jnp.float32)
    a_c, b_c = rglru_coeffs(xc_c, w_r[0], b_r[0], w_i[0], b_i[0], lam[0])
    a_x, b_x = rglru_coeffs(xc_x, w_r[0], b_r[0], w_i[0], b_i[0], lam[0])
    hc_f = linear_scan(a_c, b_c, h0)
    hx_f = linear_scan(a_x, b_x, hc_f[:, -1])
    a_c, b_c = rglru_coeffs(xc_c, w_r[1], b_r[1], w_i[1], b_i[1], lam[1])
    a_x, b_x = rglru_coeffs(xc_x, w_r[1], b_r[1], w_i[1], b_i[1], lam[1])
    hc_b = linear_scan(jnp.flip(a_c, 1), jnp.flip(b_c, 1), h0)
    hx_b = linear_scan(jnp.flip(a_x, 1), jnp.flip(b_x, 1), hc_b[:, -1])
    return hx_f + jnp.flip(hx_b, 1), hc_f + jnp.flip(hc_b, 1)


def merge_branches(ya, yb, yc, gates, w_br_na, w_br_gq, w_br_lru, w_o):
    g = jax.nn.sigmoid(gates.reshape(gates.shape[:-1] + (N_BRANCH, D_MODEL)))
    merged = g[..., 0, :] * (ya @ w_br_na) + g[..., 1, :] * (yb @ w_br_gq) + g[..., 2, :] * (yc @ w_br_lru)
    return merged @ w_o


def token_mixers(u_x, u_c, need_ctx, w_in, rpb, q_gain, k_gain, conv_w, conv_b, w_r, b_r, w_i, b_i, lam,
                 w_br_na, w_br_gq, w_br_lru, w_o):
    px = split_cols(u_x @ w_in)
    pc = split_cols(u_c @ w_in)
    na_q, na_k, na_v = heads(px[0], NA_HEADS), heads(px[1], NA_HEADS), heads(px[2], NA_HEADS)
    cna_q, cna_k, cna_v = heads(pc[0], NA_HEADS), heads(pc[1], NA_HEADS), heads(pc[2], NA_HEADS)
    ya_x = natten_latent(na_q, na_k, na_v, cna_k, cna_v, rpb)
    gq_q = rope_2d(rms_norm(heads(px[3], GQ_HEADS), q_gain))
    gq_k = rope_2d(rms_norm(heads(px[4], GQ_KV_HEADS), k_gain))
    gq_v = heads(px[5], GQ_KV_HEADS)
    cgq_q = rms_norm(heads(pc[3], GQ_HEADS), q_gain)
    cgq_k = rms_norm(heads(pc[4], GQ_KV_HEADS), k_gain)
    cgq_v = heads(pc[5], GQ_KV_HEADS)
    k_all = jnp.concatenate([gq_k, cgq_k], axis=1)
    v_all = jnp.concatenate([gq_v, cgq_v], axis=1)
    yb_x = blocked_attention(gq_q, k_all, v_all)
    xc_x = centred_conv(px[6], conv_w, conv_b)
    xc_c = centred_conv(pc[6], conv_w, conv_b)
    h_x, h_c = bidirectional_rglru(xc_x, xc_c, w_r, b_r, w_i, b_i, lam)
    yc_x = h_x.astype(u_x.dtype) * jax.nn.gelu(px[7])
    y_x = merge_branches(ya_x, yb_x, yc_x, px[8], w_br_na, w_br_gq, w_br_lru, w_o)
    if not need_ctx:
        return y_x, None
    ya_c = attend(cna_q, cna_k, cna_v)
    yb_c = attend(cgq_q, cgq_k, cgq_v)
    yc_c = h_c.astype(u_c.dtype) * jax.nn.gelu(pc[7])
    y_c = merge_branches(ya_c, yb_c, yc_c, pc[8], w_br_na, w_br_gq, w_br_lru, w_o)
    return y_x, y_c


def swiglu(u, w1, w3, w2):
    return (jax.nn.silu(u @ w1) * (u @ w3)) @ w2


def moe_swiglu(u, router, w1, w3, w2):
    logits = (u @ router).astype(jnp.float32)
    top_v, top_i = lax.top_k(logits, TOP_K)
    top_w = jax.nn.softmax(top_v, axis=-1)
    gates = jnp.sum(jax.nn.one_hot(top_i, N_EXPERTS, dtype=jnp.float32) * top_w[..., None], axis=-2).astype(u.dtype)
    out = jnp.zeros_like(u)
    for e in range(N_EXPERTS):
        out = out + gates[..., e:e + 1] * swiglu(u, w1[e], w3[e], w2[e])
    return out


def setup_inputs(seed: int = 0) -> dict:
    key = jax.random.key(seed)
    ks = jax.random.split(key, 32)

    def nrm(k, shape, scale):
        return jax.random.normal(k, shape, jnp.float32) * scale

    u = jax.random.uniform(ks[17], (DEPTH, 2, LRU_WIDTH), jnp.float32, minval=0.9, maxval=0.999)
    a0 = u ** (1.0 / LRU_C)
    return {
        'x': nrm(ks[0], (BATCH, SEQ, D_MODEL), 1.0),
        'c': nrm(ks[1], (BATCH, D_MODEL), 1.0),
        'ctx': nrm(ks[2], (BATCH, CTX_LEN, D_MODEL), 1.0),
        'c_ctx': nrm(ks[3], (D_MODEL,), 1.0),
        'w_ada': nrm(ks[4], (DEPTH, D_MODEL, 6 * D_MODEL), 0.5 * D_MODEL ** -0.5),
        'b_ada': nrm(ks[5], (DEPTH, 6 * D_MODEL), 0.01),
        'w_in': nrm(ks[6], (DEPTH, D_MODEL, IN_COLS), D_MODEL ** -0.5),
        'na_rpb': nrm(ks[7], (DEPTH, NA_HEADS, 2 * NA_WIN_H - 1, 2 * NA_WIN_W - 1), 0.2),
        'gq_q_gain': 1.0 + nrm(ks[8], (DEPTH, HEAD_DIM), 0.05),
        'gq_k_gain': 1.0 + nrm(ks[9], (DEPTH, HEAD_DIM), 0.05),
        'lru_conv_w': nrm(ks[10], (DEPTH, CONV_W, LRU_WIDTH), CONV_W ** -0.5),
        'lru_conv_b': nrm(ks[11], (DEPTH, LRU_WIDTH), 0.01),
        'lru_w_r': nrm(ks[12], (DEPTH, 2, LRU_BLOCKS, LRU_BLOCK, LRU_BLOCK), LRU_BLOCK ** -0.5),
        'lru_b_r': nrm(ks[13], (DEPTH, 2, LRU_WIDTH), 0.01),
        'lru_w_i': nrm(ks[14], (DEPTH, 2, LRU_BLOCKS, LRU_BLOCK, LRU_BLOCK), LRU_BLOCK ** -0.5),
        'lru_b_i': nrm(ks[15], (DEPTH, 2, LRU_WIDTH), 0.01),
        'lru_lambda': jnp.log(a0) - jnp.log1p(-a0),
        'w_br_na': nrm(ks[18], (DEPTH, NA_WIDTH, D_MODEL), NA_WIDTH ** -0.5),
        'w_br_gq': nrm(ks[19], (DEPTH, GQ_Q_WIDTH, D_MODEL), GQ_Q_WIDTH ** -0.5),
        'w_br_lru': nrm(ks[20], (DEPTH, LRU_WIDTH, D_MODEL), LRU_WIDTH ** -0.5),
        'w_o': nrm(ks[21], (DEPTH, D_MODEL, D_MODEL), BETA * D_MODEL ** -0.5),
        'ln1_g': 1.0 + nrm(ks[22], (DEPTH, D_MODEL), 0.05),
        'ln1_b': nrm(ks[23], (DEPTH, D_MODEL), 0.01),
        'ln2_g': 1.0 + nrm(ks[24], (DEPTH, D_MODEL), 0.05),
        'ln2_b': nrm(ks[25], (DEPTH, D_MODEL), 0.01),
        'ffn_w1': nrm(ks[26], (N_DENSE, D_MODEL, D_FF), D_MODEL ** -0.5),
        'ffn_w3': nrm(ks[27], (N_DENSE, D_MODEL, D_FF), D_MODEL ** -0.5),
        'ffn_w2': nrm(ks[28], (N_DENSE, D_FF, D_MODEL), BETA * D_FF ** -0.5),
        'moe_router': nrm(ks[29], (N_MOE, D_MODEL, N_EXPERTS), D_MODEL ** -0.5),
        'moe_w1': nrm(ks[30], (N_MOE, N_EXPERTS, D_MODEL, D_FF_EXPERT), D_MODEL ** -0.5),
        'moe_w3': nrm(ks[31], (N_MOE, N_EXPERTS, D_MODEL, D_FF_EXPERT), D_MODEL ** -0.5),
        'moe_w2': nrm(ks[16], (N_MOE, N_EXPERTS, D_FF_EXPERT, D_MODEL), BETA * D_FF_EXPERT ** -0.5),
    }


def reference(x, c, ctx, c_ctx, w_ada, b_ada, w_in, na_rpb, gq_q_gain, gq_k_gain, lru_conv_w, lru_conv_b,
              lru_w_r, lru_b_r, lru_w_i, lru_b_i, lru_lambda, w_br_na, w_br_gq, w_br_lru, w_o,
              ln1_g, ln1_b, ln2_g, ln2_b, ffn_w1, ffn_w3, ffn_w2, moe_router, moe_w1, moe_w3, moe_w2):
    B = x.shape[0]
    silu_c = jax.nn.silu(c)
    silu_cc = jax.nn.silu(c_ctx)
    for l in range(DEPTH):
        need_ctx = l < DEPTH - 1
        mod_x = (silu_c @ w_ada[l] + b_ada[l]).reshape(B, 6, 1, D_MODEL)
        mod_c = (silu_cc @ w_ada[l] + b_ada[l]).reshape(6, 1, D_MODEL)
        u_x = x * (1.0 + mod_x[:, 1]) + mod_x[:, 0]
        u_c = ctx * (1.0 + mod_c[1]) + mod_c[0]
        y_x, y_c = token_mixers(u_x, u_c, need_ctx, w_in[l], na_rpb[l], gq_q_gain[l], gq_k_gain[l],
                                lru_conv_w[l], lru_conv_b[l], lru_w_r[l], lru_b_r[l], lru_w_i[l], lru_b_i[l],
                                lru_lambda[l], w_br_na[l], w_br_gq[l], w_br_lru[l], w_o[l])
        x = layer_norm(ALPHA * x + mod_x[:, 2] * y_x, ln1_g[l], ln1_b[l])
        u2_x = x * (1.0 + mod_x[:, 4]) + mod_x[:, 3]
        if l % 2 == 0:
            f_x = swiglu(u2_x, ffn_w1[l // 2], ffn_w3[l // 2], ffn_w2[l // 2])
        else:
            f_x = moe_swiglu(u2_x, moe_router[l // 2], moe_w1[l // 2], moe_w3[l // 2], moe_w2[l // 2])
        x = layer_norm(ALPHA * x + mod_x[:, 5] * f_x, ln2_g[l], ln2_b[l])
        if need_ctx:
            ctx = layer_norm(ALPHA * ctx + mod_c[2] * y_c, ln1_g[l], ln1_b[l])
            u2_c = ctx * (1.0 + mod_c[4]) + mod_c[3]
            if l % 2 == 0:
                f_c = swiglu(u2_c, ffn_w1[l // 2], ffn_w3[l // 2], ffn_w2[l // 2])
            else:
                f_c = moe_swiglu(u2_c, moe_router[l // 2], moe_w1[l // 2], moe_w3[l // 2], moe_w2[l // 2])
            ctx = layer_norm(ALPHA * ctx + mod_c[5] * f_c, ln2_g[l], ln2_b[l])
    return x
```

```python
import numpy as np
import concourse.bass as bass
import concourse.mybir as mybir
from concourse.bass_utils import run_bass_kernel_spmd

F32 = mybir.dt.float32
BF16 = mybir.dt.bfloat16
AF = mybir.ActivationFunctionType
ALU = mybir.AluOpType
AX = mybir.AxisListType


class Buf:
    __slots__ = ("name", "kind", "w", "r", "slot")

    def __init__(self, name, kind="strict"):
        self.name = name
        self.kind = kind
        self.w = {}
        self.r = {}


class Eng:
    def __init__(self, K, name, eng, sem):
        self.K, self.name, self.eng, self.sem = K, name, eng, sem
        self.count = 0
        self.waited = {}

    def wait(self, tok):
        sem, val, key = tok
        if key.startswith("d_"):
            val = max(val, self.K.slots[key[2:]][1])
        if self.waited.get(key, 0) >= val:
            return
        if key == "pe" and self.name == "pe":
            return
        self.eng.wait_ge(sem, val)
        self.waited[key] = val


class Kern:
    def __init__(self, nc, stack):
        self.nc = nc
        self.stack = stack
        self.nsem = 0
        self.pe = self._eng("pe", nc.tensor)
        self.act = self._eng("act", nc.scalar)
        self.dve = self._eng("dve", nc.vector)
        self.pool = self._eng("pool", nc.gpsimd)
        self.sp = self._eng("sp", nc.sync)
        self.slots = {}
        self.cur = stack

    def _sem(self, name):
        self.nsem += 1
        return self.stack.enter_context(self.nc.semaphore(name))

    def _eng(self, name, eng):
        return Eng(self, name, eng, self._sem("s_" + name))

    def sbuf(self, name, shape, dt):
        self.uid = getattr(self, "uid", 0) + 1
        return self.cur.enter_context(self.nc.sbuf_tensor(f"{name}_{self.uid}", shape, dt))

    def psum(self, name, shape, dt):
        self.uid = getattr(self, "uid", 0) + 1
        return self.cur.enter_context(self.nc.psum_tensor(f"{name}_{self.uid}", shape, dt))

    def _deps(self, E, reads, writes):
        for b in reads:
            for t in b.w.values():
                E.wait(t)
        for b in writes:
            if b.kind == "strict":
                for t in b.w.values():
                    E.wait(t)
            for t in b.r.values():
                E.wait(t)

    def _mark(self, tok, reads, writes):
        key = tok[2]
        for b in reads:
            b.r[key] = tok
        for b in writes:
            if b.kind != "strict":
                b.w[key] = tok
            else:
                b.w = {key: tok}
                b.r = {}

    def op(self, E, fn, reads=(), writes=()):
        self._deps(E, reads, writes)
        ins = fn()
        E.count += 1
        ins.then_inc(E.sem, 1)
        tok = (E.sem, E.count, E.name)
        self._mark(tok, reads, writes)
        return tok

    def mm_group(self, mms, reads, out):
        E = self.pe
        self._deps(E, reads, [out])
        ins = None
        for fn in mms:
            ins = fn()
        E.count += 1
        ins.then_inc(E.sem, 1)
        tok = (E.sem, E.count, E.name)
        self._mark(tok, reads, [out])
        return tok

    def dma(self, Q, slot, out_ap, in_ap, reads=(), writes=(), **kw):
        if slot not in self.slots:
            self.slots[slot] = [self._sem("d_" + slot), 0]
        self._deps(Q, reads, writes)
        s = self.slots[slot]
        ins = Q.eng.dma_start(out=out_ap, in_=in_ap, **kw)
        s[1] += 16
        ins.then_inc(s[0], 16)
        tok = (s[0], s[1], "d_" + slot)
        self._mark(tok, reads, writes)
        return tok

    def finish(self, bufs):
        for b in bufs:
            for t in b.w.values():
                self.sp.wait(t)


    def barrier(self):
        engs = [self.pe, self.act, self.dve, self.pool, self.sp]
        toks = [(e.sem, e.count, e.name) for e in engs if e.count > 0]
        toks += [(s[0], s[1], "d_" + k) for k, s in self.slots.items() if s[1] > 0]
        for e in engs:
            for t in toks:
                if t[2] != e.name:
                    e.wait(t)


from contextlib import ExitStack, contextmanager

D = 4096
SEQ = 4096
CTX = 256
T = SEQ + CTX
DEPTH = 2
GW = 64
NAH, GQH, GKV = 12, 16, 4
NAW, GQW, GKW, LW = 1536, 2048, 512, 1536
IN_COLS = 23040
DFF, NEXP, DFE = 11008, 8, 3072
ALPHA = (2 * DEPTH) ** 0.25
EPS = 1e-6
SCALE = 128 ** -0.5
BLOCKS = [(i * 512, 512) for i in range(8)] + [(4096, 256)]
KC = D // 128


@contextmanager
def phase(K):
    K.barrier()
    old = K.cur
    with ExitStack() as st:
        K.cur = st
        yield
        K.barrier()
    K.cur = old


class Pools:
    def __init__(self, K, welems=8192, nw=3, nps=7, psh=False):
        self.K = K
        self.welems = welems
        self.wt = [K.sbuf(f"wslot{i}", [128, welems], BF16) for i in range(nw)]
        self.wbuf = [Buf(f"wslot{i}") for i in range(nw)]
        self.nw = nw
        self.wi = 0
        self.ps = [K.psum(f"ps{i}", [128, 512], F32) for i in range(nps)]
        self.psb = [Buf(f"ps{i}") for i in range(nps)]
        if psh:
            self.psh = K.psum("psh", [128, 1024], BF16)
            self.pshb = Buf("psh")

    def next_w(self):
        i = self.wi
        self.wi = (self.wi + 1) % self.nw
        return i


def gemm(K, P, *, pieces, SK, M, MW, xt, xbuf, n, groups, epi, banks, pre=None, m_base=0):
    nc = K.nc
    assert SK * MW <= P.welems, (SK, MW)
    pi = 0
    for m0 in range(0, M, MW):
        mw = min(MW, M - m0)
        si = P.next_w()
        wv = P.wt[si][:, 0:SK * mw].rearrange("p (k m) -> p k m", k=SK)
        for (k0, nk, ap) in pieces(m0, mw):
            K.dma(K.pool, f"w{si}", wv[:, k0:k0 + nk, :], ap.rearrange("(k p) m -> p k m", p=128), writes=[P.wbuf[si]])
        for mi in range(0, mw, 128):
            mc = (m_base + m0 + mi) // 128
            if pre is not None:
                pre(mc)
            bk = banks[pi % len(banks)]
            pi += 1
            outs, obufs = [], []
            for gi, (ks0, ks1, xk0) in enumerate(groups):
                o = P.ps[bk[gi]][:, 0:n]
                fns = []
                for ks in range(ks0, ks1):
                    fns.append(lambda o=o, ks=ks, xk=xk0 + ks - ks0, a=(ks == ks0), z=(ks == ks1 - 1):
                               nc.tensor.matmul(o, wv[:, ks, mi:mi + 128], xt[:, xk, 0:n], start=a, stop=z))
                K.mm_group(fns, reads=[P.wbuf[si], xbuf], out=P.psb[bk[gi]])
                outs.append(o)
                obufs.append(P.psb[bk[gi]])
            epi(mc, outs, obufs)


def gemm_tm(K, P, *, pieces, SK, M, MW, xt, xbuf, n, epi, banks):
    nc = K.nc
    pi = 0
    for m0 in range(0, M, MW):
        mw = min(MW, M - m0)
        si = P.next_w()
        wv = P.wt[si][:, 0:SK * mw].rearrange("p (k m) -> p k m", k=SK)
        for (k0, nk, ap) in pieces(m0, mw):
            K.dma(K.pool, f"w{si}", wv[:, k0:k0 + nk, :], ap.rearrange("(k p) m -> p k m", p=128), writes=[P.wbuf[si]])
        for tt in range(n // 128):
            bk = banks[pi % len(banks)]
            pi += 1
            o = P.ps[bk][:, 0:mw]
            fns = [lambda o=o, ks=ks, tt=tt: nc.tensor.matmul(o, xt[:, ks, tt * 128:(tt + 1) * 128], wv[:, ks, :], start=(ks == 0), stop=(ks == SK - 1))
                   for ks in range(SK)]
            K.mm_group(fns, reads=[P.wbuf[si], xbuf], out=P.psb[bk])
            epi(m0, mw, tt, o, P.psb[bk])


def gemm2(K, P, *, srcs, KT, M, xt, xbuf, n, after, pre=None, MG=512):
    nc = K.nc
    KS = P.welems // MG
    for m0 in range(0, M, MG):
        mg = min(MG, M - m0)
        nch = mg // 128
        if pre is not None:
            for j in range(nch):
                pre(m0 // 128 + j)
        for (src, banks) in srcs:
            for ks0 in range(0, KT, KS):
                ks1 = min(KT, ks0 + KS)
                si = P.next_w()
                wv = P.wt[si][:, 0:(ks1 - ks0) * mg].rearrange("p (k m) -> p k m", m=mg)
                K.dma(K.pool, f"w{si}", wv, src[ks0 * 128:ks1 * 128, m0:m0 + mg].rearrange("(k p) m -> p k m", p=128), writes=[P.wbuf[si]])
                for j in range(nch):
                    o = P.ps[banks[j]][:, 0:n]
                    fns = [lambda k=k, o=o, j=j: nc.tensor.matmul(o, wv[:, k - ks0, j * 128:(j + 1) * 128], xt[:, k, 0:n], start=(k == 0), stop=(k == KT - 1))
                           for k in range(ks0, ks1)]
                    K.mm_group(fns, reads=[P.wbuf[si], xbuf], out=P.psb[banks[j]])
        after(m0 // 128, nch)


class Rot:
    def __init__(self, K, name, shape, dt, n, slot=None):
        self.t = [K.sbuf(f"{name}{i}", shape, dt) for i in range(n)]
        self.b = [Buf(f"{name}{i}") for i in range(n)]
        for i, b in enumerate(self.b):
            b.slot = f"{slot or name}{i}"
        self.i = 0

    def next(self):
        i = self.i
        self.i = (self.i + 1) % len(self.t)
        return self.t[i], self.b[i]


def rs_of(r):
    return min(max(r - 4, 0), 56)


def build(nlayers=DEPTH, debug=(), stop_after=None):
    nc = bass.Bass("TRN2", target_bir_lowering=False)

    def din(name, shape, dt=F32):
        return nc.dram_tensor(name, list(shape), dt, kind="ExternalInput").ap()

    def scr(name, shape, dt):
        if name in debug:
            return nc.dram_tensor(name, list(shape), dt, kind="ExternalOutput").ap()
        return nc.dram_tensor(name, list(shape), dt).ap()

    xin = din("xin", [T, D])
    cvec = din("cvec", [128, 32, 2])
    ident_d = din("ident", [128, 128])
    cos_d = din("ropec", [128, T])
    sin_d = din("ropes", [128, T])
    rotm_d = din("rotm", [128, 128])
    esel_d = din("esel", [8, 1024])
    nmask_d = din("nmask", [128, 14 * 64])
    L = []
    for l in range(nlayers):
        w = {}
        w["w_ada"] = din(f"w_ada{l}", [D, 6 * D])
        w["bada"] = din(f"bada{l}", [128, 192])
        w["w_in"] = din(f"w_in{l}", [D, IN_COLS])
        w["bt"] = din(f"bt{l}", [128, NAH, 14 * 64])
        w["gains"] = din(f"gains{l}", [128, 2])
        w["convw"] = din(f"convw{l}", [128, 12, 4])
        w["convb"] = din(f"convb{l}", [128, 12])
        w["w_r"] = din(f"lru_w_r{l}", [2, 12, 128, 128])
        w["w_i"] = din(f"lru_w_i{l}", [2, 12, 128, 128])
        w["lrub"] = din(f"lrub{l}", [128, 2, 12, 2])
        w["lam"] = din(f"lam{l}", [128, 2, 12])
        w["w_br_na"] = din(f"w_br_na{l}", [NAW, D])
        w["w_br_gq"] = din(f"w_br_gq{l}", [GQW, D])
        w["w_br_lru"] = din(f"w_br_lru{l}", [LW, D])
        w["w_o"] = din(f"w_o{l}", [D, D])
        w["lnp"] = din(f"lnp{l}", [128, 4, 32])
        if l % 2 == 0:
            w["ffn_w1"] = din(f"ffn_w1_{l}", [D, DFF])
            w["ffn_w3"] = din(f"ffn_w3_{l}", [D, DFF])
            w["ffn_w2"] = din(f"ffn_w2_{l}", [DFF, D])
        else:
            w["router"] = din(f"router{l}", [128, 32, 8])
            w["moe_w1"] = [din(f"moe_w1_{l}_{e}", [D, DFE]) for e in range(NEXP)]
            w["moe_w3"] = [din(f"moe_w3_{l}_{e}", [D, DFE]) for e in range(NEXP)]
            w["moe_w2"] = [din(f"moe_w2_{l}_{e}", [DFE, D]) for e in range(NEXP)]
        L.append(w)
    out_d = nc.dram_tensor("out", [SEQ, D], F32, kind="ExternalOutput").ap()

    XR = scr("XR", [D, T], F32)
    NAQ = scr("NAQ", [NAW, T], BF16)
    NAK = scr("NAK", [NAW, T], BF16)
    NAV = scr("NAV", [T, NAW], BF16)
    GQQ = scr("GQQ", [GQW, T], BF16)
    GQK = scr("GQK", [GKW, T], BF16)
    GQV = scr("GQV", [T, GKW], BF16)
    LX = scr("LX", [LW, T], F32)
    LG = scr("LG", [LW, T], F32)
    SG = scr("SG", [3 * D, T], BF16)
    YA = scr("YA", [NAW, T], BF16)
    YB = scr("YB", [GQW, T], BF16)
    YC = scr("YC", [LW, T], BF16)
    XRB = [[Buf(f"XR{b}_{k}", "dram") for k in range(KC)] for b in range(9)]
    sB = {n: Buf(n, "dram") for n in ["NAQ", "NAK", "NAV", "GQQ", "GQK", "GQV", "LX", "LG", "SG", "YA", "YB", "YC", "out"]}

    with ExitStack() as st:
        K = Kern(nc, st)
        identf = K.sbuf("identf", [128, 128], F32); identB = Buf("identf")
        K.dma(K.sp, "c0", identf[:], ident_d, writes=[identB])
        onesf = K.sbuf("onesf", [128, 128], F32); onesfB = Buf("onesf")
        K.op(K.dve, lambda: nc.vector.memset(onesf[:], 1.0), writes=[onesfB])
        onesb = K.sbuf("onesb", [128, 128], BF16); onesbB = Buf("onesb")
        K.op(K.dve, lambda: nc.vector.memset(onesb[:], 1.0), writes=[onesbB])
        cst = K.sbuf("cst", [128, 4], F32); cstB = Buf("cst")
        K.op(K.dve, lambda: nc.vector.memset(cst[:, 0:1], EPS), writes=[cstB])
        K.op(K.dve, lambda: nc.vector.memset(cst[:, 1:2], 1.0), writes=[cstB])
        K.op(K.dve, lambda: nc.vector.memset(cst[:, 2:4], 0.0), writes=[cstB])
        mod = K.sbuf("mod", [128, 192, 2], F32); modB = Buf("mod", "multi")
        lnp = K.sbuf("lnp", [128, 4, 32], F32); lnpB = Buf("lnp")

        def vcopy(E, out, in_, reads, writes):
            if E is K.act:
                return K.op(E, lambda: nc.scalar.copy(out=out, in_=in_), reads, writes)
            return K.op(E, lambda: E.eng.tensor_copy(out=out, in_=in_), reads, writes)

        def vtt(out, a, b, op, reads, writes, E=None):
            E = E or K.dve
            return K.op(E, lambda: E.eng.tensor_tensor(out=out, in0=a, in1=b, op=op), reads, writes)

        def actf(out, in_, func, reads, writes, **kw):
            return K.op(K.act, lambda: nc.scalar.activation(out=out, in_=in_, func=func, **kw), reads, writes)

        def phase0():
            with phase(K):
                P = Pools(K, welems=16, nw=1, nps=4)
                xrow = Rot(K, "xrow", [128, D], F32, 2, slot="lr")
                stg = Rot(K, "stg0", [128, 32, 128], F32, 2, slot="sa")
                for b_ in stg.b:
                    b_.kind = "multi"
                for tt in range(T // 128):
                    xr, xrb = xrow.next()
                    K.dma(K.sp, xrb.slot, xr[:], xin[tt * 128:(tt + 1) * 128, :], writes=[xrb])
                    sg, sgb = stg.next()
                    for q in range(8):
                        bk = q % 4
                        K.mm_group([lambda j=j: nc.tensor.transpose(P.ps[bk][:, j * 128:(j + 1) * 128], xr[:, (4 * q + j) * 128:(4 * q + j + 1) * 128], identf[:])
                                    for j in range(4)], reads=[xrb, identB], out=P.psb[bk])
                        vcopy(K.act if q % 2 == 0 else K.dve, sg[:, 4 * q:4 * q + 4, :], P.ps[bk][:, 0:512].rearrange("p (j t) -> p j t", j=4),
                              [P.psb[bk]], [sgb])
                    blk = min(tt // 4, 8)
                    K.dma(K.sp, sgb.slot, XR[:, tt * 128:(tt + 1) * 128].rearrange("(k p) t -> p k t", p=128), sg[:], reads=[sgb], writes=XRB[blk])

        def phase_mod(l):
            W = L[l]
            with phase(K):
                P = Pools(K, welems=16384, nw=3, nps=4)
                cv = K.sbuf("cv", [128, 32, 2], F32); cvB = Buf("cv")
                K.dma(K.sp, "ldA", cv[:], cvec, writes=[cvB])
                cb = K.sbuf("cb", [128, 32, 2], BF16); cbB = Buf("cb")
                actf(cb[:], cv[:], AF.Silu, [cvB], [cbB])
                ba = K.sbuf("ba", [128, 192], F32); baB = Buf("ba")
                K.dma(K.sp, "ldA", ba[:], W["bada"], writes=[baB])
                K.dma(K.sp, "ldA", lnp[:], W["lnp"], writes=[lnpB])

                def epi(mc, outs, obufs):
                    K.op(K.dve, lambda: nc.vector.tensor_scalar(out=mod[:, mc, :], in0=outs[0], scalar1=ba[:, mc:mc + 1], scalar2=None, op0=ALU.add),
                         reads=[obufs[0], baB], writes=[modB])
                gemm(K, P, pieces=lambda m0, mw: [(0, 32, W["w_ada"][:, m0:m0 + mw])], SK=32, M=6 * D, MW=512, xt=cb, xbuf=cbB, n=2,
                     groups=[(0, 32, 0)], epi=epi, banks=[[0], [1], [2], [3]])
                for j in (1, 4):
                    K.op(K.dve, lambda: nc.vector.tensor_scalar(out=mod[:, j * 32:(j + 1) * 32, :], in0=mod[:, j * 32:(j + 1) * 32, :], scalar1=1.0, scalar2=None, op0=ALU.add),
                         reads=[modB], writes=[modB])

        def build_u(bi, xt, xtB, xin_rot, jshift, jscale, extra=None):
            t0, n = BLOCKS[bi]
            col = 0 if bi < 8 else 1
            for k in range(KC):
                xi, xib = xin_rot.next()
                K.dma(K.sp, xib.slot, xi[:, 0:n], XR[k * 128:(k + 1) * 128, t0:t0 + n], reads=[XRB[bi][k]], writes=[xib])
                if extra is None:
                    K.op(K.dve, lambda: nc.vector.tensor_scalar(out=xt[:, k, 0:n], in0=xi[:, 0:n], scalar1=mod[:, jscale * 32 + k, col:col + 1],
                                                                scalar2=mod[:, jshift * 32 + k, col:col + 1], op0=ALU.mult, op1=ALU.add),
                         reads=[xib, modB], writes=[xtB])
                else:
                    extra(k, xi, xib, n, col)

        def phase_in(l):
            W = L[l]
            with phase(K):
                P = Pools(K, welems=16384, nw=3, nps=7)
                xt = K.sbuf("xtB", [128, KC, 512], BF16); xtB = Buf("xt")
                xin_rot = Rot(K, "xinB", [128, 512], F32, 3, slot="lx")
                sgb_ = Rot(K, "stgb", [128, 512], BF16, 4, slot="sa")
                sgf_ = Rot(K, "stgf", [128, 512], F32, 4, slot="sb")
                tmp = Rot(K, "tmpB", [128, 512], F32, 6)
                cs = K.sbuf("cs", [128, 2, 512], F32); csB = Buf("cs")
                gn = K.sbuf("gn", [128, 2], F32); gnB = Buf("gn")
                rotm = K.sbuf("rotm", [128, 128], F32); rotB = Buf("rotm")
                K.dma(K.sp, "ldc", gn[:], W["gains"], writes=[gnB])
                K.dma(K.sp, "ldc", rotm[:], rotm_d, writes=[rotB])
                w_in = W["w_in"]
                nbl = 9

                for bi in range(nbl):
                    t0, n = BLOCKS[bi]
                    build_u(bi, xt, xtB, xin_rot, 0, 1)
                    K.dma(K.sp, "ldc", cs[:, 0, 0:n], cos_d[:, t0:t0 + n], writes=[csB])
                    K.dma(K.sp, "ldc", cs[:, 1, 0:n], sin_d[:, t0:t0 + n], writes=[csB])

                    def store(dst, rows0, src, srcB, name):
                        K.dma(K.sp, srcB.slot, dst[rows0:rows0 + 128, t0:t0 + n], src, reads=[srcB], writes=[sB[name]])

                    def epi(mc, outs, obufs):
                        ps, pb = outs[0], obufs[0]
                        if mc < 24:
                            s, sb_ = sgb_.next()
                            vcopy(K.act, s[:, 0:n], ps, [pb], [sb_])
                            if mc < 12:
                                store(NAQ, mc * 128, s[:, 0:n], sb_, "NAQ")
                            else:
                                store(NAK, (mc - 12) * 128, s[:, 0:n], sb_, "NAK")
                        elif 36 <= mc < 56:
                            isq = mc < 52
                            sq, sqB = tmp.next()
                            actf(sq[:, 0:n], ps, AF.Square, [pb], [sqB])
                            K.mm_group([lambda: nc.tensor.matmul(P.ps[4][:, 0:n], onesf[:], sq[:, 0:n], start=True, stop=True)], reads=[sqB, onesfB], out=P.psb[4])
                            sd, sdB = tmp.next()
                            actf(sd[:, 0:n], P.ps[4][:, 0:n], AF.Sqrt, [P.psb[4], cstB], [sdB], scale=1.0 / 128, bias=cst[:, 0:1])
                            rsd, rsdB = tmp.next()
                            K.op(K.dve, lambda: nc.vector.reciprocal(out=rsd[:, 0:n], in_=sd[:, 0:n]), [sdB], [rsdB])
                            qn, qnB = tmp.next()
                            gcol = 0 if isq else 1
                            K.op(K.dve, lambda: nc.vector.scalar_tensor_tensor(out=qn[:, 0:n], in0=ps, scalar=gn[:, gcol:gcol + 1], in1=rsd[:, 0:n], op0=ALU.mult, op1=ALU.mult),
                                 [pb, gnB, rsdB], [qnB])
                            K.mm_group([lambda: nc.tensor.matmul(P.ps[5][:, 0:n], rotm[:], qn[:, 0:n], start=True, stop=True)], reads=[qnB, rotB], out=P.psb[5])
                            t1, t1B = tmp.next()
                            vtt(t1[:, 0:n], qn[:, 0:n], cs[:, 0, 0:n], ALU.mult, [qnB, csB], [t1B])
                            t2, t2B = tmp.next()
                            vtt(t2[:, 0:n], P.ps[5][:, 0:n], cs[:, 1, 0:n], ALU.mult, [P.psb[5], csB], [t2B])
                            s, sb_ = sgb_.next()
                            vtt(s[:, 0:n], t1[:, 0:n], t2[:, 0:n], ALU.add, [t1B, t2B], [sb_])
                            if isq:
                                store(GQQ, (mc - 36) * 128, s[:, 0:n], sb_, "GQQ")
                            else:
                                store(GQK, (mc - 52) * 128, s[:, 0:n], sb_, "GQK")
                        elif 60 <= mc < 72:
                            s, sb_ = sgf_.next()
                            vcopy(K.act, s[:, 0:n], ps, [pb], [sb_])
                            store(LX, (mc - 60) * 128, s[:, 0:n], sb_, "LX")
                        elif 72 <= mc < 84:
                            s, sb_ = sgf_.next()
                            actf(s[:, 0:n], ps, AF.Gelu, [pb], [sb_])
                            store(LG, (mc - 72) * 128, s[:, 0:n], sb_, "LG")
                        else:
                            assert mc >= 84
                            s, sb_ = sgb_.next()
                            actf(s[:, 0:n], ps, AF.Sigmoid, [pb], [sb_])
                            store(SG, (mc - 84) * 128, s[:, 0:n], sb_, "SG")

                    def epi_v(dst, name, c0):
                        def f(m0, mw, tt, o, ob):
                            s, sb_ = sgb_.next()
                            vcopy(K.dve, s[:, 0:mw], o, [ob], [sb_])
                            K.dma(K.sp, sb_.slot, dst[t0 + tt * 128:t0 + (tt + 1) * 128, c0 + m0:c0 + m0 + mw], s[:, 0:mw], reads=[sb_], writes=[sB[name]])
                        return f

                    def run_fm(c0, c1):
                        gemm(K, P, pieces=lambda m0, mw: [(0, 32, w_in[:, c0 + m0:c0 + m0 + mw])], SK=32, M=c1 - c0, MW=512, xt=xt, xbuf=xtB, n=n,
                             groups=[(0, 32, 0)], epi=epi, banks=[[0], [1], [2], [3]], m_base=c0)

                    def run_tm(c0, c1, dst, name):
                        gemm_tm(K, P, pieces=lambda m0, mw: [(0, 32, w_in[:, c0 + m0:c0 + m0 + mw])], SK=32, M=c1 - c0, MW=512, xt=xt, xbuf=xtB, n=n,
                                epi=epi_v(dst, name, 0), banks=[0, 1, 2, 3])

                    if bi == 8 and l == DEPTH - 1:
                        run_fm(1536, 3072)
                        run_tm(3072, 4608, NAV, "NAV")
                        run_fm(6656, 7168)
                        run_tm(7168, 7680, GQV, "GQV")
                        run_fm(7680, 9216)
                    else:
                        run_fm(0, 3072)
                        run_tm(3072, 4608, NAV, "NAV")
                        run_fm(4608, 7168)
                        run_tm(7168, 7680, GQV, "GQV")
                        run_fm(7680, IN_COLS)

        def phase_gqa(l):
            nqb = 9 if l < DEPTH - 1 else 8
            with phase(K):
                P = Pools(K, welems=16, nw=1, nps=4)
                Kt = K.sbuf("Kt", [128, T], BF16); KtB = Buf("Kt")
                Vt = K.sbuf("Vt", [128, 34, 128], BF16); VtB = Buf("Vt")
                Qr = Rot(K, "Qt", [128, T], BF16, 2, slot="lq")
                Pr = Rot(K, "Pt", [128, 512], BF16, 4)
                rd = Rot(K, "rdg", [128, 512], F32, 2)
                so = Rot(K, "sog", [128, 512], BF16, 2, slot="sa")
                for g in range(GKV):
                    K.dma(K.sp, "ldg", Kt[:], GQK[g * 128:(g + 1) * 128, :], reads=[sB["GQK"]], writes=[KtB])
                    K.dma(K.sp, "ldg", Vt[:], GQV[:, g * 128:(g + 1) * 128].rearrange("(c p) d -> p c d", p=128), reads=[sB["GQV"]], writes=[VtB])
                    for hh in range(4):
                        h = g * 4 + hh
                        Qt, QtB = Qr.next()
                        K.dma(K.sp, QtB.slot, Qt[:], GQQ[h * 128:(h + 1) * 128, :], reads=[sB["GQQ"]], writes=[QtB])
                        for bi in range(nqb):
                            t0, n = BLOCKS[bi]
                            kcs = list(range(34)) if bi < 8 else [32, 33]

                            def S(i):
                                kc = kcs[i]
                                bk = i % 2
                                K.mm_group([lambda: nc.tensor.matmul(P.ps[bk][:, 0:n], Kt[:, kc * 128:(kc + 1) * 128], Qt[:, t0:t0 + n], start=True, stop=True)],
                                           reads=[KtB, QtB], out=P.psb[bk])
                            S(0)
                            for i, kc in enumerate(kcs):
                                bk = i % 2
                                pt, ptB = Pr.next()
                                actf(pt[:, 0:n], P.ps[bk][:, 0:n], AF.Exp, [P.psb[bk]], [ptB], scale=SCALE)
                                if i + 1 < len(kcs):
                                    S(i + 1)
                                a, z = (i == 0), (i == len(kcs) - 1)
                                K.mm_group([lambda: nc.tensor.matmul(P.ps[2][:, 0:n], Vt[:, kc, :], pt[:, 0:n], start=a, stop=z)], reads=[VtB, ptB], out=P.psb[2])
                                K.mm_group([lambda: nc.tensor.matmul(P.ps[3][:, 0:n], onesb[:], pt[:, 0:n], start=a, stop=z)], reads=[onesbB, ptB], out=P.psb[3])
                            r_, rB = rd.next()
                            K.op(K.dve, lambda: nc.vector.reciprocal(out=r_[:, 0:n], in_=P.ps[3][:, 0:n]), [P.psb[3]], [rB])
                            s, sb_ = so.next()
                            vtt(s[:, 0:n], P.ps[2][:, 0:n], r_[:, 0:n], ALU.mult, [P.psb[2], rB], [sb_])
                            K.dma(K.sp, sb_.slot, YB[h * 128:(h + 1) * 128, t0:t0 + n], s[:, 0:n], reads=[sb_], writes=[sB["YB"]])

        def phase_na(l):
            W = L[l]
            with phase(K):
                P = Pools(K, welems=16, nw=1, nps=6)
                BT = K.sbuf("BT", [128, NAH, 14 * 64], F32); BTB = Buf("BT")
                nm = K.sbuf("nm", [128, 14 * 64], F32); nmB = Buf("nm")
                K.dma(K.sp, "ldn", BT[:], W["bt"], writes=[BTB])
                K.dma(K.sp, "ldn", nm[:], nmask_d, writes=[nmB])
                for h in range(NAH):
                    vtt(BT[:, h, :], BT[:, h, :], nm[:], ALU.add, [BTB, nmB], [BTB])
                Kc = K.sbuf("Kc", [128, NAH, CTX], BF16); KcB = Buf("Kc")
                Vc = K.sbuf("Vc", [128, 2, NAW], BF16); VcB = Buf("Vc")
                K.dma(K.sp, "ldn", Kc[:], NAK[:, SEQ:T].rearrange("(h p) t -> p h t", p=128), reads=[sB["NAK"]], writes=[KcB])
                K.dma(K.sp, "ldn", Vc[:], NAV[SEQ:T, :].rearrange("(c p) d -> p c d", p=128), reads=[sB["NAV"]], writes=[VcB])
                Qg = K.sbuf("Qg", [128, NAH, 512], BF16); QgB = Buf("Qg")
                Kw = K.sbuf("Kw", [128, NAH, 1024], BF16); KwB = Buf("Kw")
                Ve = K.sbuf("Ve", [128, 8, NAW], BF16); VeB = Buf("Ve")
                Vo = K.sbuf("Vo", [128, 7, NAW], BF16); VoB = Buf("Vo")
                Pc = Rot(K, "Pc", [128, 2, 512], BF16, 2)
                sbf = Rot(K, "sbf", [128, 4, 64], F32, 3)
                Pw = Rot(K, "Pw", [128, 4, 64], BF16, 3)
                rd = Rot(K, "rdn", [128, 512], F32, 2)
                so = Rot(K, "son", [128, 512], BF16, 2, slot="sa")

                def finish(h, tq0, n):
                    r_, rB = rd.next()
                    K.op(K.dve, lambda: nc.vector.reciprocal(out=r_[:, 0:n], in_=P.ps[5][:, 0:n]), [P.psb[5]], [rB])
                    s, sb_ = so.next()
                    vtt(s[:, 0:n], P.ps[4][:, 0:n], r_[:, 0:n], ALU.mult, [P.psb[4], rB], [sb_])
                    K.dma(K.sp, sb_.slot, YA[h * 128:(h + 1) * 128, tq0:tq0 + n], s[:, 0:n], reads=[sb_], writes=[sB["YA"]])

                def ctx_scores(h, n):
                    pc, pcB = Pc.next()
                    for c in range(2):
                        K.mm_group([lambda: nc.tensor.matmul(P.ps[c][:, 0:n], Kc[:, h, c * 128:(c + 1) * 128], Qg[:, h, 0:n], start=True, stop=True)],
                                   reads=[KcB, QgB], out=P.psb[c])
                        actf(pc[:, c, 0:n], P.ps[c][:, 0:n], AF.Exp, [P.psb[c]], [pcB], scale=SCALE)
                    return pc, pcB

                for rg in range(8):
                    U0 = rs_of(8 * rg)
                    tok0 = U0 * 64
                    K.dma(K.sp, "ldn", Qg[:], NAQ[:, rg * 512:(rg + 1) * 512].rearrange("(h p) t -> p h t", p=128), reads=[sB["NAQ"]], writes=[QgB])
                    K.dma(K.sp, "ldn", Kw[:], NAK[:, tok0:tok0 + 1024].rearrange("(h p) t -> p h t", p=128), reads=[sB["NAK"]], writes=[KwB])
                    K.dma(K.sp, "ldn", Ve[:], NAV[tok0:tok0 + 1024, :].rearrange("(c p) d -> p c d", p=128), reads=[sB["NAV"]], writes=[VeB])
                    K.dma(K.sp, "ldn", Vo[:], NAV[tok0 + 64:tok0 + 64 + 896, :].rearrange("(c p) d -> p c d", p=128), reads=[sB["NAV"]], writes=[VoB])
                    for h in range(NAH):
                        hc = slice(h * 128, (h + 1) * 128)
                        pc, pcB = ctx_scores(h, 512)
                        for rl in range(8):
                            r = 8 * rg + rl
                            off0 = rs_of(r) - U0
                            i0 = rs_of(r) - r + 7
                            bk = 2 + (rl % 2)
                            K.mm_group([lambda j=j: nc.tensor.matmul(P.ps[bk][:, j * 64:(j + 1) * 64], Kw[:, h, (off0 + 2 * j) * 64:(off0 + 2 * j) * 64 + 128],
                                                                     Qg[:, h, rl * 64:(rl + 1) * 64], start=True, stop=True) for j in range(4)],
                                       reads=[KwB, QgB], out=P.psb[bk])
                            sf, sfB = sbf.next()
                            bsl = BT[:, h, :].rearrange("p (a i q) -> p a i q", a=2, i=7)[:, i0 % 2, i0 // 2:i0 // 2 + 4, :]
                            K.op(K.dve, lambda: nc.vector.scalar_tensor_tensor(out=sf[:], in0=P.ps[bk][:, 0:256].rearrange("p (j q) -> p j q", j=4), scalar=SCALE,
                                                                               in1=bsl, op0=ALU.mult, op1=ALU.add), [P.psb[bk], BTB], [sfB])
                            pw, pwB = Pw.next()
                            actf(pw[:], sf[:], AF.Exp, [sfB], [pwB])
                            for (lhs_ones, obk) in ((False, 4), (True, 5)):
                                fns = []
                                for j in range(4):
                                    off = off0 + 2 * j
                                    V = Ve[:, off // 2, hc] if off % 2 == 0 else Vo[:, (off - 1) // 2, hc]
                                    fns.append(lambda j=j, V=V: nc.tensor.matmul(P.ps[obk][:, rl * 64:(rl + 1) * 64], onesb[:] if lhs_ones else V, pw[:, j, :], start=(j == 0), stop=False))
                                for c in range(2):
                                    fns.append(lambda c=c: nc.tensor.matmul(P.ps[obk][:, rl * 64:(rl + 1) * 64], onesb[:] if lhs_ones else Vc[:, c, hc], pc[:, c, rl * 64:(rl + 1) * 64],
                                                                            start=False, stop=(c == 1)))
                                K.mm_group(fns, reads=[VeB, VoB, VcB, pwB, pcB, onesbB], out=P.psb[obk])
                        finish(h, rg * 512, 512)
                if l < DEPTH - 1:
                    K.dma(K.sp, "ldn", Qg[:, :, 0:CTX], NAQ[:, SEQ:T].rearrange("(h p) t -> p h t", p=128), reads=[sB["NAQ"]], writes=[QgB])
                    for h in range(NAH):
                        hc = slice(h * 128, (h + 1) * 128)
                        pc, pcB = ctx_scores(h, CTX)
                        for (lhs_ones, obk) in ((False, 4), (True, 5)):
                            K.mm_group([lambda c=c: nc.tensor.matmul(P.ps[obk][:, 0:CTX], onesb[:] if lhs_ones else Vc[:, c, hc], pc[:, c, 0:CTX], start=(c == 0), stop=(c == 1))
                                        for c in range(2)], reads=[VcB, pcB, onesbB], out=P.psb[obk])
                        finish(h, SEQ, CTX)

        def rev(a, ln):
            return bass.AP(a.tensor, a.offset + ln - 1, [list(a.ap[0]), [-1, ln]])

        def phase_lru(l):
            W = L[l]
            with phase(K):
                P = Pools(K, welems=16, nw=1, nps=4)
                wg = K.sbuf("wg", [128, 2, 2, 12, 128], BF16); wgB = Buf("wg")
                K.dma(K.pool, "ldl", wg[:, 0], W["w_r"].rearrange("d g i j -> i d g j"), writes=[wgB])
                K.dma(K.pool, "ldl", wg[:, 1], W["w_i"].rearrange("d g i j -> i d g j"), writes=[wgB])
                cw = K.sbuf("cw", [128, 12, 4], F32); cwB = Buf("cw")
                cbi = K.sbuf("cbi", [128, 12], F32); cbB = Buf("cbi")
                lb = K.sbuf("lb", [128, 2, 12, 2], F32); lbB = Buf("lb")
                lam = K.sbuf("lam", [128, 24], F32); lamB = Buf("lam")
                K.dma(K.sp, "ldl2", cw[:], W["convw"], writes=[cwB])
                K.dma(K.sp, "ldl2", cbi[:], W["convb"], writes=[cbB])
                K.dma(K.sp, "ldl2", lb[:], W["lrub"], writes=[lbB])
                K.dma(K.sp, "ldl2", lam[:], W["lam"].rearrange("p d g -> p (d g)"), writes=[lamB])
                e_ = K.sbuf("e_", [128, 24], F32); eB = Buf("e_")
                z_ = K.sbuf("z_", [128, 24], F32); zB = Buf("z_")
                z2 = K.sbuf("z2", [128, 24], F32); z2B = Buf("z2")
                pl = K.sbuf("pl", [128, 24], F32); plB = Buf("pl")
                negc = K.sbuf("negc", [128, 24], F32); ncB = Buf("negc")
                actf(e_[:], lam[:], AF.Exp, [lamB], [eB], scale=-1.0)
                K.op(K.dve, lambda: nc.vector.tensor_scalar(out=z_[:], in0=e_[:], scalar1=2.0, scalar2=None, op0=ALU.add), [eB], [zB])
                K.op(K.dve, lambda: nc.vector.reciprocal(out=z_[:], in_=z_[:]), [zB], [zB])
                vtt(z_[:], z_[:], e_[:], ALU.mult, [zB, eB], [zB])
                vtt(z2[:], z_[:], z_[:], ALU.mult, [zB], [z2B])
                K.op(K.dve, lambda: nc.vector.tensor_scalar(out=pl[:], in0=z2[:], scalar1=1.0 / 7, scalar2=1.0 / 5, op0=ALU.mult, op1=ALU.add), [z2B], [plB])
                vtt(pl[:], pl[:], z2[:], ALU.mult, [plB, z2B], [plB])
                K.op(K.dve, lambda: nc.vector.tensor_scalar(out=pl[:], in0=pl[:], scalar1=1.0 / 3, scalar2=None, op0=ALU.add), [plB], [plB])
                vtt(pl[:], pl[:], z2[:], ALU.mult, [plB, z2B], [plB])
                K.op(K.dve, lambda: nc.vector.tensor_scalar(out=pl[:], in0=pl[:], scalar1=1.0, scalar2=None, op0=ALU.add), [plB], [plB])
                vtt(pl[:], pl[:], z_[:], ALU.mult, [plB, zB], [plB])
                K.op(K.dve, lambda: nc.vector.tensor_scalar(out=negc[:], in0=pl[:], scalar1=-16.0, scalar2=None, op0=ALU.mult), [plB], [ncB])

                def big(name, dt=F32):
                    return K.sbuf(name, [128, T], dt), Buf(name)
                xl, xlB = big("xl"); xc, xcB = big("xc"); xcb, xcbB = big("xcb", BF16); lg, lgB = big("lg"); hs, hsB = big("hs")
                gr, grB = big("gr"); gi, giB = big("gi"); a2, a2B = big("a2"); hd, hdB = big("hd")
                segs = [(0, SEQ), (SEQ, CTX)]
                for g in range(12):
                    K.dma(K.sp, "ldl", xl[:], LX[g * 128:(g + 1) * 128, :], reads=[sB["LX"]], writes=[xlB])
                    K.dma(K.sp, "ldl", lg[:], LG[g * 128:(g + 1) * 128, :], reads=[sB["LG"]], writes=[lgB])
                    actf(xc[:], xl[:], AF.Identity, [xlB, cwB, cbB], [xcB], scale=cw[:, g, 2:3], bias=cbi[:, g:g + 1])
                    for (s0, ln) in segs:
                        for tap, sh in ((0, 2), (1, 1)):
                            K.op(K.dve, lambda: nc.vector.scalar_tensor_tensor(out=xc[:, s0 + sh:s0 + ln], in0=xl[:, s0:s0 + ln - sh], scalar=cw[:, g, tap:tap + 1],
                                                                               in1=xc[:, s0 + sh:s0 + ln], op0=ALU.mult, op1=ALU.add), [xlB, cwB, xcB], [xcB])
                        K.op(K.dve, lambda: nc.vector.scalar_tensor_tensor(out=xc[:, s0:s0 + ln - 1], in0=xl[:, s0 + 1:s0 + ln], scalar=cw[:, g, 3:4],
                                                                           in1=xc[:, s0:s0 + ln - 1], op0=ALU.mult, op1=ALU.add), [xlB, cwB, xcB], [xcB])
                    vcopy(K.act, xcb[:], xc[:], [xcB], [xcbB])
                    for d in range(2):
                        for bi, (t0, n) in enumerate(BLOCKS):
                            for ri, (dst, dstB) in enumerate(((gr, grB), (gi, giB))):
                                bk = (2 * bi + ri) % 4
                                K.mm_group([lambda: nc.tensor.matmul(P.ps[bk][:, 0:n], wg[:, ri, d, g, :], xcb[:, t0:t0 + n], start=True, stop=True)],
                                           reads=[wgB, xcbB], out=P.psb[bk])
                                actf(dst[:, t0:t0 + n], P.ps[bk][:, 0:n], AF.Sigmoid, [P.psb[bk], lbB], [dstB], bias=lb[:, d, g, ri:ri + 1])
                        actf(gr[:], gr[:], AF.Exp, [grB, ncB], [grB], scale=negc[:, d * 12 + g:d * 12 + g + 1])
                        vtt(a2[:], gr[:], gr[:], ALU.mult, [grB], [a2B])
                        actf(a2[:], a2[:], AF.Sqrt, [a2B, cstB], [a2B], scale=-1.0, bias=cst[:, 1:2])
                        vtt(gi[:], gi[:], xc[:], ALU.mult, [giB, xcB], [giB])
                        vtt(a2[:], a2[:], gi[:], ALU.mult, [a2B, giB], [a2B])
                        tgt, tgtB = (hs, hsB) if d == 0 else (hd, hdB)
                        if d == 0:
                            K.op(K.dve, lambda: nc.vector.tensor_tensor_scan(out=tgt[:, SEQ:T], data0=gr[:, SEQ:T], data1=a2[:, SEQ:T], initial=0.0, op0=ALU.mult, op1=ALU.add),
                                 [grB, a2B], [tgtB])
                            K.op(K.dve, lambda: nc.vector.tensor_tensor_scan(out=tgt[:, 0:SEQ], data0=gr[:, 0:SEQ], data1=a2[:, 0:SEQ], initial=tgt[:, T - 1:T], op0=ALU.mult, op1=ALU.add),
                                 [grB, a2B, tgtB], [tgtB])
                        else:
                            K.op(K.dve, lambda: nc.vector.tensor_tensor_scan(out=rev(tgt[:, SEQ:T], CTX), data0=rev(gr[:, SEQ:T], CTX), data1=rev(a2[:, SEQ:T], CTX), initial=0.0,
                                                                             op0=ALU.mult, op1=ALU.add), [grB, a2B], [tgtB])
                            K.op(K.dve, lambda: nc.vector.tensor_tensor_scan(out=rev(tgt[:, 0:SEQ], SEQ), data0=rev(gr[:, 0:SEQ], SEQ), data1=rev(a2[:, 0:SEQ], SEQ),
                                                                             initial=tgt[:, SEQ:SEQ + 1], op0=ALU.mult, op1=ALU.add), [grB, a2B, tgtB], [tgtB])
                            vtt(hs[:], hs[:], hd[:], ALU.add, [hsB, hdB], [hsB])
                    vtt(xcb[:], hs[:], lg[:], ALU.mult, [hsB, lgB], [xcbB])
                    K.dma(K.sp, "stl", YC[g * 128:(g + 1) * 128, :], xcb[:], reads=[xcbB], writes=[sB["YC"]])

        def resid(bi, jgate, xin_rot, stg, src_of=None):
            t0, n = BLOCKS[bi]
            col = 0 if bi < 8 else 1
            cur = {}

            def pre(mc):
                xi, xiB = xin_rot.next()
                K.dma(K.sp, xiB.slot, xi[:, 0:n], XR[mc * 128:(mc + 1) * 128, t0:t0 + n], reads=[XRB[bi][mc]], writes=[xiB])
                cur[mc] = (xi, xiB)

            def epi(mc, outs, obufs):
                xi, xiB = cur.pop(mc)
                actf(xi[:, 0:n], xi[:, 0:n], AF.Identity, [xiB, cstB], [xiB], scale=ALPHA, bias=cst[:, 2:3])
                s, sb_ = stg.next()
                K.op(K.dve, lambda: nc.vector.scalar_tensor_tensor(out=s[:, 0:n], in0=outs[0], scalar=mod[:, jgate * 32 + mc, col:col + 1], in1=xi[:, 0:n],
                                                                   op0=ALU.mult, op1=ALU.add), [obufs[0], modB, xiB], [sb_])
                K.dma(K.sp, sb_.slot, XR[mc * 128:(mc + 1) * 128, t0:t0 + n], s[:, 0:n], reads=[sb_], writes=[XRB[bi][mc]])
            return pre, epi

        def phase_merge(l):
            W = L[l]
            nbl = 9 if l < DEPTH - 1 else 8
            with phase(K):
                P = Pools(K, welems=16384, nw=2, nps=7)
                ycat = K.sbuf("ycat", [128, 40, 512], BF16); ycB = Buf("ycat")
                mg = K.sbuf("mg", [128, 32, 512], BF16); mgB = Buf("mg")
                sgt = Rot(K, "sgt", [128, 3, 512], BF16, 2, slot="lg")
                tq = Rot(K, "tD", [128, 512], F32, 4)
                xin_rot = Rot(K, "xinD", [128, 512], F32, 3, slot="lx")
                stg = Rot(K, "stgD", [128, 512], F32, 3, slot="sb")
                SGv = SG.rearrange("(i f) t -> f i t", i=3)
                for bi in range(nbl):
                    t0, n = BLOCKS[bi]
                    K.dma(K.sp, "ldy", ycat[:, 0:12, 0:n], YA[:, t0:t0 + n].rearrange("(k p) t -> p k t", p=128), reads=[sB["YA"]], writes=[ycB])
                    K.dma(K.sp, "ldy", ycat[:, 12:28, 0:n], YB[:, t0:t0 + n].rearrange("(k p) t -> p k t", p=128), reads=[sB["YB"]], writes=[ycB])
                    K.dma(K.sp, "ldy", ycat[:, 28:40, 0:n], YC[:, t0:t0 + n].rearrange("(k p) t -> p k t", p=128), reads=[sB["YC"]], writes=[ycB])
                    cur = {}

                    def pre(mc):
                        s, sb_ = sgt.next()
                        K.dma(K.sp, sb_.slot, s[:, :, 0:n], SGv[mc * 128:(mc + 1) * 128, :, t0:t0 + n], reads=[sB["SG"]], writes=[sb_])
                        cur[mc] = (s, sb_)

                    def epi(mc, outs, obufs):
                        s, sb_ = cur.pop(mc)
                        ta, taB = tq.next()
                        tb, tbB = tq.next()
                        vtt(ta[:, 0:n], outs[0], s[:, 0, 0:n], ALU.mult, [obufs[0], sb_], [taB])
                        vtt(tb[:, 0:n], outs[1], s[:, 1, 0:n], ALU.mult, [obufs[1], sb_], [tbB])
                        vtt(ta[:, 0:n], ta[:, 0:n], tb[:, 0:n], ALU.add, [taB, tbB], [taB])
                        vtt(tb[:, 0:n], outs[2], s[:, 2, 0:n], ALU.mult, [obufs[2], sb_, taB], [tbB])
                        vtt(mg[:, mc, 0:n], ta[:, 0:n], tb[:, 0:n], ALU.add, [taB, tbB], [mgB])
                    gemm(K, P, pieces=lambda m0, mw: [(0, 12, W["w_br_na"][:, m0:m0 + mw]), (12, 16, W["w_br_gq"][:, m0:m0 + mw]), (28, 12, W["w_br_lru"][:, m0:m0 + mw])],
                         SK=40, M=D, MW=384, xt=ycat, xbuf=ycB, n=n, groups=[(0, 12, 0), (12, 28, 12), (28, 40, 28)], epi=epi, pre=pre, banks=[[0, 1, 2], [3, 4, 5]])
                    pre2, epi2 = resid(bi, 2, xin_rot, stg)
                    gemm(K, P, pieces=lambda m0, mw: [(0, 32, W["w_o"][:, m0:m0 + mw])], SK=32, M=D, MW=512, xt=mg, xbuf=mgB, n=n, groups=[(0, 32, 0)],
                         epi=epi2, pre=pre2, banks=[[0], [1], [2], [3]])

        def phase_ln(l, which, final=False):
            nbl = 9 if l < DEPTH - 1 else 8
            with phase(K):
                P = Pools(K, welems=16, nw=1, nps=4)
                zb = Rot(K, "zb", [128, 32, 512], F32, 1 if final else 2, slot="lz")
                sq = Rot(K, "sqL", [128, 512], F32, 3)
                tq = Rot(K, "tL", [128, 512], F32, 4)
                stg = Rot(K, "stgL", [128, 512], F32, 3, slot="sb")
                mean, meanB = K.sbuf("mean", [128, 512], F32), Buf("mean")
                msq, msqB = K.sbuf("msq", [128, 512], F32), Buf("msq")
                rstd, rstdB = K.sbuf("rstd", [128, 512], F32), Buf("rstd")
                nmr, nmrB = K.sbuf("nmr", [128, 512], F32), Buf("nmr")
                if final:
                    rowbuf = K.sbuf("rowbuf", [128, 4, D], F32); rowB = Buf("rowbuf", "multi")
                for bi in range(nbl):
                    t0, n = BLOCKS[bi]
                    z, zB = zb.next()
                    K.dma(K.sp, zB.slot, z[:, :, 0:n], XR[:, t0:t0 + n].rearrange("(k p) t -> p k t", p=128), reads=XRB[bi], writes=[zB])
                    K.mm_group([lambda k=k: nc.tensor.matmul(P.ps[0][:, 0:n], onesf[:], z[:, k, 0:n], start=(k == 0), stop=(k == KC - 1)) for k in range(KC)],
                               reads=[zB, onesfB], out=P.psb[0])
                    for k in range(KC):
                        s_, sqB = sq.next()
                        actf(s_[:, 0:n], z[:, k, 0:n], AF.Square, [zB], [sqB])
                        K.mm_group([lambda: nc.tensor.matmul(P.ps[1][:, 0:n], onesf[:], s_[:, 0:n], start=(k == 0), stop=(k == KC - 1))], reads=[sqB, onesfB], out=P.psb[1])
                    K.op(K.dve, lambda: nc.vector.tensor_scalar(out=mean[:, 0:n], in0=P.ps[0][:, 0:n], scalar1=1.0 / D, scalar2=None, op0=ALU.mult), [P.psb[0]], [meanB])
                    vtt(msq[:, 0:n], mean[:, 0:n], mean[:, 0:n], ALU.mult, [meanB], [msqB])
                    K.op(K.dve, lambda: nc.vector.scalar_tensor_tensor(out=msq[:, 0:n], in0=P.ps[1][:, 0:n], scalar=1.0 / D, in1=msq[:, 0:n], op0=ALU.mult, op1=ALU.subtract),
                         [P.psb[1], msqB], [msqB])
                    actf(msq[:, 0:n], msq[:, 0:n], AF.Sqrt, [msqB, cstB], [msqB], scale=1.0, bias=cst[:, 0:1])
                    K.op(K.dve, lambda: nc.vector.reciprocal(out=rstd[:, 0:n], in_=msq[:, 0:n]), [msqB], [rstdB])
                    K.op(K.dve, lambda: nc.vector.scalar_tensor_tensor(out=nmr[:, 0:n], in0=mean[:, 0:n], scalar=-1.0, in1=rstd[:, 0:n], op0=ALU.mult, op1=ALU.mult),
                         [meanB, rstdB], [nmrB])
                    for k in range(KC):
                        ta, taB = tq.next()
                        vtt(ta[:, 0:n], z[:, k, 0:n], rstd[:, 0:n], ALU.mult, [zB, rstdB], [taB])
                        vtt(ta[:, 0:n], ta[:, 0:n], nmr[:, 0:n], ALU.add, [taB, nmrB], [taB])
                        s, sb_ = stg.next()
                        actf(s[:, 0:n], ta[:, 0:n], AF.Identity, [taB, lnpB], [sb_], scale=lnp[:, 2 * which, k:k + 1], bias=lnp[:, 2 * which + 1, k:k + 1])
                        if not final:
                            K.dma(K.sp, sb_.slot, XR[k * 128:(k + 1) * 128, t0:t0 + n], s[:, 0:n], reads=[sb_], writes=[XRB[bi][k]])
                        else:
                            bk = 2 + k % 2
                            K.mm_group([lambda j=j: nc.tensor.transpose(P.ps[bk][:, j * 128:(j + 1) * 128], s[:, j * 128:(j + 1) * 128], identf[:]) for j in range(n // 128)],
                                       reads=[sb_, identB], out=P.psb[bk])
                            vcopy(K.act if k % 2 == 0 else K.dve, rowbuf[:, 0:n // 128, k * 128:(k + 1) * 128], P.ps[bk][:, 0:n].rearrange("p (j d) -> p j d", d=128),
                                  [P.psb[bk]], [rowB])
                    if final:
                        K.dma(K.sp, "sto", out_d[t0:t0 + n, :].rearrange("(j p) d -> p j d", p=128), rowbuf[:, 0:n // 128, :], reads=[rowB], writes=[sB["out"]])

        def phase_ffn(l):
            W = L[l]
            nbl = 9 if l < DEPTH - 1 else 8
            with phase(K):
                P = Pools(K, welems=12288, nw=2, nps=8)
                xt = K.sbuf("xtE", [128, KC, 512], BF16); xtB = Buf("xtE")
                hh = K.sbuf("hE", [128, 86, 512], BF16); hB = Buf("hE", "multi")
                xin_rot = Rot(K, "xinE", [128, 512], F32, 5, slot="lx")
                tq = Rot(K, "tE", [128, 512], F32, 4)
                stg = Rot(K, "stgE", [128, 512], F32, 3, slot="sb")
                for bi in range(nbl):
                    t0, n = BLOCKS[bi]
                    build_u(bi, xt, xtB, xin_rot, 3, 4)

                    def after1(mc0, nch):
                        for j in range(nch):
                            ta, taB = tq.next()
                            actf(ta[:, 0:n], P.ps[j][:, 0:n], AF.Silu, [P.psb[j]], [taB])
                            vtt(hh[:, mc0 + j, 0:n], ta[:, 0:n], P.ps[4 + j][:, 0:n], ALU.mult, [taB, P.psb[4 + j]], [hB])
                    gemm2(K, P, srcs=[(W["ffn_w1"], [0, 1, 2, 3]), (W["ffn_w3"], [4, 5, 6, 7])], KT=32, M=DFF, xt=xt, xbuf=xtB, n=n, after=after1)
                    pre2, epi2 = resid(bi, 5, xin_rot, stg)

                    def after2(mc0, nch):
                        for j in range(nch):
                            epi2(mc0 + j, [P.ps[j][:, 0:n]], [P.psb[j]])
                    gemm2(K, P, srcs=[(W["ffn_w2"], [0, 1, 2, 3])], KT=86, M=D, xt=hh, xbuf=hB, n=n, after=after2, pre=pre2)

        def phase_moe(l):
            W = L[l]
            nbl = 9 if l < DEPTH - 1 else 8
            with phase(K):
                P = Pools(K, welems=12288, nw=2, nps=8)
                xt = K.sbuf("xtM", [128, KC, 512], BF16); xtB = Buf("xtM")
                hh = K.sbuf("hM", [128, 24, 512], BF16); hB = Buf("hM", "multi")
                facc = K.sbuf("facc", [128, KC, 512], F32); faccB = [Buf(f"facc{k}") for k in range(KC)]
                rt = K.sbuf("rt", [128, 32, 8], F32); rtB = Buf("rt")
                es = K.sbuf("es", [8, 1024], F32); esB = Buf("es")
                K.dma(K.sp, "ldc", rt[:], W["router"], writes=[rtB])
                K.dma(K.sp, "ldc", es[:], esel_d, writes=[esB])
                xin_rot = Rot(K, "xinM", [128, 512], F32, 3, slot="lx")
                u2f = Rot(K, "u2f", [128, 512], F32, 2)
                tq = Rot(K, "tM", [128, 512], F32, 3)
                stg = Rot(K, "stgM", [128, 512], F32, 2, slot="sb")
                Gb = Rot(K, "Gb", [128, 512], F32, 2)
                lg8 = K.sbuf("lg8", [8, 512], F32); lg8B = Buf("lg8")
                gT = K.sbuf("gT", [8, 512], F32); gTB = Buf("gT")
                lt = K.sbuf("lt", [128, 4, 8], F32); ltB = Buf("lt")
                m8 = K.sbuf("m8", [128, 4, 8], F32); m8B = Buf("m8")
                nv1 = K.sbuf("nv1", [128, 4, 1], F32); nv1B = Buf("nv1")
                msk = K.sbuf("msk", [128, 4, 8], F32); mskB = Buf("msk")
                ex = K.sbuf("ex", [128, 4, 8], F32); exB = Buf("ex")
                den = K.sbuf("den", [128, 4, 1], F32); denB = Buf("den")
                gt = K.sbuf("gt", [128, 4, 8], F32); gtB = Buf("gt")
                for bi in range(nbl):
                    t0, n = BLOCKS[bi]
                    nt = n // 128

                    def extra(k, xi, xib, n, col):
                        uf, ufB = u2f.next()
                        K.op(K.dve, lambda: nc.vector.tensor_scalar(out=uf[:, 0:n], in0=xi[:, 0:n], scalar1=mod[:, 4 * 32 + k, col:col + 1],
                                                                    scalar2=mod[:, 3 * 32 + k, col:col + 1], op0=ALU.mult, op1=ALU.add), [xib, modB], [ufB])
                        vcopy(K.act, xt[:, k, 0:n], uf[:, 0:n], [ufB], [xtB])
                        K.mm_group([lambda: nc.tensor.matmul(P.ps[6][0:8, 0:n], rt[:, k, :], uf[:, 0:n], start=(k == 0), stop=(k == KC - 1))], reads=[rtB, ufB], out=P.psb[6])
                    build_u(bi, xt, xtB, xin_rot, 3, 4, extra=extra)
                    vcopy(K.dve, lg8[:, 0:n], P.ps[6][0:8, 0:n], [P.psb[6]], [lg8B])
                    K.mm_group([lambda j=j: nc.tensor.transpose(P.ps[5][:, j * 8:(j + 1) * 8], lg8[0:8, j * 128:(j + 1) * 128], identf[0:8, 0:8]) for j in range(nt)],
                               reads=[lg8B, identB], out=P.psb[5])
                    vcopy(K.dve, lt[:, 0:nt, :], P.ps[5][:, 0:nt * 8].rearrange("p (j e) -> p j e", e=8), [P.psb[5]], [ltB])
                    for j in range(nt):
                        K.op(K.dve, lambda: nc.vector.max(out=m8[:, j, :], in_=lt[:, j, :]), [ltB], [m8B])
                    K.op(K.dve, lambda: nc.vector.tensor_scalar(out=nv1[:, 0:nt, :], in0=m8[:, 0:nt, 0:1], scalar1=-1.0, scalar2=None, op0=ALU.mult), [m8B], [nv1B])
                    for j in range(nt):
                        K.op(K.dve, lambda: nc.vector.tensor_scalar(out=msk[:, j, :], in0=lt[:, j, :], scalar1=m8[:, j, 1:2], scalar2=None, op0=ALU.is_ge), [ltB, m8B], [mskB])
                        actf(ex[:, j, :], lt[:, j, :], AF.Exp, [ltB, nv1B], [exB], bias=nv1[:, j, :], scale=1.0)
                    vtt(ex[:, 0:nt, :], ex[:, 0:nt, :], msk[:, 0:nt, :], ALU.mult, [exB, mskB], [exB])
                    K.op(K.dve, lambda: nc.vector.reduce_sum(out=den[:, 0:nt, :], in_=ex[:, 0:nt, :], axis=AX.X), [exB], [denB])
                    K.op(K.dve, lambda: nc.vector.reciprocal(out=den[:, 0:nt, :], in_=den[:, 0:nt, :]), [denB], [denB])
                    for j in range(nt):
                        K.op(K.dve, lambda: nc.vector.tensor_scalar(out=gt[:, j, :], in0=ex[:, j, :], scalar1=den[:, j, :], scalar2=None, op0=ALU.mult), [exB, denB], [gtB])
                    K.mm_group([lambda j=j: nc.tensor.transpose(P.ps[5][0:8, j * 128:(j + 1) * 128], gt[:, j, :], identf[:]) for j in range(nt)],
                               reads=[gtB, identB], out=P.psb[5])
                    vcopy(K.dve, gT[:, 0:n], P.ps[5][0:8, 0:n], [P.psb[5]], [gTB])
                    for e in range(NEXP):
                        K.mm_group([lambda: nc.tensor.matmul(P.ps[6][:, 0:n], es[0:8, e * 128:(e + 1) * 128], gT[0:8, 0:n], start=True, stop=True)], reads=[esB, gTB], out=P.psb[6])
                        gb, gbB = Gb.next()
                        vcopy(K.dve, gb[:, 0:n], P.ps[6][:, 0:n], [P.psb[6]], [gbB])

                        def after1(mc0, nch):
                            for j in range(nch):
                                ta, taB = tq.next()
                                actf(ta[:, 0:n], P.ps[j][:, 0:n], AF.Silu, [P.psb[j]], [taB])
                                vtt(ta[:, 0:n], ta[:, 0:n], P.ps[4 + j][:, 0:n], ALU.mult, [taB, P.psb[4 + j]], [taB])
                                vtt(hh[:, mc0 + j, 0:n], ta[:, 0:n], gb[:, 0:n], ALU.mult, [taB, gbB], [hB])
                        gemm2(K, P, srcs=[(W["moe_w1"][e], [0, 1, 2, 3]), (W["moe_w3"][e], [4, 5, 6, 7])], KT=32, M=DFE, xt=xt, xbuf=xtB, n=n, after=after1)

                        def after2(mc0, nch):
                            for j in range(nch):
                                mc = mc0 + j
                                if e == 0:
                                    vcopy(K.act, facc[:, mc, 0:n], P.ps[j][:, 0:n], [P.psb[j]], [faccB[mc]])
                                else:
                                    vtt(facc[:, mc, 0:n], facc[:, mc, 0:n], P.ps[j][:, 0:n], ALU.add, [faccB[mc], P.psb[j]], [faccB[mc]])
                        gemm2(K, P, srcs=[(W["moe_w2"][e], [0, 1, 2, 3])], KT=24, M=D, xt=hh, xbuf=hB, n=n, after=after2)
                    pre2, epi2r = resid(bi, 5, xin_rot, stg)
                    for mc in range(KC):
                        pre2(mc)
                        epi2r(mc, [facc[:, mc, 0:n]], [faccB[mc]])

        phase0()
        done = stop_after == "p0"
        for l in range(nlayers):
            if done:
                break
            for name, fn in (("mod", lambda: phase_mod(l)), ("in", lambda: phase_in(l)), ("na", lambda: phase_na(l)), ("gqa", lambda: phase_gqa(l)),
                             ("lru", lambda: phase_lru(l)), ("merge", lambda: phase_merge(l)), ("ln1", lambda: phase_ln(l, 0)),
                             ("ffn", lambda: (phase_ffn(l) if l % 2 == 0 else phase_moe(l))), ("ln2", lambda: phase_ln(l, 1, final=(l == DEPTH - 1)))):
                fn()
                if stop_after == f"{name}{l}":
                    done = True
                    break
        K.barrier()
        K.finish([sB["out"]])
    return nc


def _consts():
    c = {}
    c["ident"] = np.eye(128, dtype=np.float32)
    p = np.arange(128)
    t = np.arange(SEQ)
    inv = (np.float32(10000.0) ** (-(np.arange(32, dtype=np.float32)) / np.float32(32))).astype(np.float32)
    pos = np.where((p < 64)[:, None], (t // GW)[None, :], (t % GW)[None, :]).astype(np.float32)
    ang = (pos * inv[p % 32][:, None]).astype(np.float32)
    cos = np.ones((128, T), np.float32)
    sin = np.zeros((128, T), np.float32)
    cos[:, :SEQ] = np.cos(ang)
    sin[:, :SEQ] = np.sin(ang)
    c["ropec"], c["ropes"] = cos, sin
    rotm = np.zeros((128, 128), np.float32)
    for base in (0, 64):
        for m in range(32):
            rotm[base + m + 32, base + m] = -1.0
            rotm[base + m, base + m + 32] = 1.0
    c["rotm"] = rotm
    esel = np.zeros((8, 8, 128), np.float32)
    for e in range(8):
        esel[e, e, :] = 1.0
    c["esel"] = esel.reshape(8, 1024)
    q = np.arange(GW)
    cs = np.clip(q - 8, 0, GW - 16)
    kc = p % 64
    valid = (kc[:, None] >= cs[None, :]) & (kc[:, None] < cs[None, :] + 16)
    nm = np.where(valid, 0.0, -30000.0).astype(np.float32)
    c["nmask"] = np.ascontiguousarray(np.broadcast_to(nm[:, None, :], (128, 14, 64))).reshape(128, 14 * 64)
    return c


def _bt_index():
    p = np.arange(128)[:, None, None, None]
    a = np.arange(2)[None, :, None, None]
    i = np.arange(7)[None, None, :, None]
    q = np.arange(64)[None, None, None, :]
    roff = (2 * i + a) + (p // 64) + 0 * q
    coff = np.clip((p % 64) - q, -15, 15) + 15 + 0 * a * i
    return np.broadcast_to(roff, (128, 2, 7, 64)), np.broadcast_to(coff, (128, 2, 7, 64))


def _fm(v, inner=None):
    v = np.asarray(v)
    lead = v.shape[:-1]
    r = v.reshape(lead + (v.shape[-1] // 128, 128))
    return np.ascontiguousarray(np.moveaxis(r, -1, 0))


def _prep(inp, b, nlayers, consts):
    m = dict(consts)
    m["xin"] = np.ascontiguousarray(np.concatenate([inp["x"][b], inp["ctx"][b]], axis=0))
    m["cvec"] = _fm(np.stack([inp["c"][b], inp["c_ctx"]], 0)).transpose(0, 2, 1).copy()
    roff, coff = _bt_index()
    for l in range(nlayers):
        m[f"w_ada{l}"] = inp["w_ada"][l]
        m[f"bada{l}"] = _fm(inp["b_ada"][l])
        m[f"w_in{l}"] = inp["w_in"][l]
        rpb = inp["na_rpb"][l]
        m[f"bt{l}"] = np.ascontiguousarray(np.moveaxis(rpb[:, roff, coff], 0, 1)).reshape(128, NAH, 14 * 64)
        m[f"gains{l}"] = np.ascontiguousarray(np.stack([inp["gq_q_gain"][l], inp["gq_k_gain"][l]], -1))
        m[f"convw{l}"] = np.ascontiguousarray(_fm(inp["lru_conv_w"][l]).transpose(0, 2, 1))
        m[f"convb{l}"] = _fm(inp["lru_conv_b"][l])
        m[f"lru_w_r{l}"] = inp["lru_w_r"][l]
        m[f"lru_w_i{l}"] = inp["lru_w_i"][l]
        m[f"lrub{l}"] = np.ascontiguousarray(np.stack([_fm(inp["lru_b_r"][l]), _fm(inp["lru_b_i"][l])], -1))
        m[f"lam{l}"] = _fm(inp["lru_lambda"][l])
        m[f"w_br_na{l}"] = inp["w_br_na"][l]
        m[f"w_br_gq{l}"] = inp["w_br_gq"][l]
        m[f"w_br_lru{l}"] = inp["w_br_lru"][l]
        m[f"w_o{l}"] = inp["w_o"][l]
        m[f"lnp{l}"] = _fm(np.stack([inp["ln1_g"][l], inp["ln1_b"][l], inp["ln2_g"][l], inp["ln2_b"][l]], 0))
        if l % 2 == 0:
            m[f"ffn_w1_{l}"] = inp["ffn_w1"][l // 2]
            m[f"ffn_w3_{l}"] = inp["ffn_w3"][l // 2]
            m[f"ffn_w2_{l}"] = inp["ffn_w2"][l // 2]
        else:
            m[f"router{l}"] = np.ascontiguousarray(inp["moe_router"][l // 2].reshape(32, 128, 8).transpose(1, 0, 2))
            for e in range(NEXP):
                m[f"moe_w1_{l}_{e}"] = inp["moe_w1"][l // 2][e]
                m[f"moe_w3_{l}_{e}"] = inp["moe_w3"][l // 2][e]
                m[f"moe_w2_{l}_{e}"] = inp["moe_w2"][l // 2][e]
    return m


def kernel(**inputs):
    inp = {k: np.asarray(v) for k, v in inputs.items()}
    nc = build()
    consts = _consts()
    maps = [_prep(inp, b, DEPTH, consts) for b in range(2)]
    res = run_bass_kernel_spmd(nc, maps, core_ids=[0, 1])
    return np.stack([np.asarray(res.results[b]["out"], dtype=np.float32) for b in range(2)], axis=0)
```
